# Optimizing a Trainium2 kernel written in Bass

```python
import math
import jax
import jax.numpy as jnp
from jax import lax
import numpy as np

D_MODEL = 4096
BATCH = 2
SEQ = 8192
DEPTH = 4

GRID_W = 64
CTX_LEN = 256
HEAD_DIM = 128
ROPE_THETA = 10000.0
NORM_EPS = 1e-6
Q_BLOCK = 128
N_MOD = 6
N_PAIRS = DEPTH // 2

A_HEADS = D_MODEL // (2 * HEAD_DIM)
A_KV_HEADS = A_HEADS // 4
A_GROUP = A_HEADS // A_KV_HEADS
A_Q = A_HEADS * HEAD_DIM
A_KV = A_KV_HEADS * HEAD_DIM

B_D_INNER = D_MODEL // 2
B_HEAD_DIM = 64
B_HEADS = B_D_INNER // B_HEAD_DIM
B_GROUPS = 4
B_HPG = B_HEADS // B_GROUPS
B_STATE = 128
B_GN = B_GROUPS * B_STATE
B_CONV_CH = B_D_INNER + 2 * B_GN
B_CONV_W = 5
B_CHUNK = 128
DT_MIN = 1e-3
DT_MAX = 1e-1

C_HEADS = D_MODEL // HEAD_DIM
C_KV_HEADS = C_HEADS // 4
C_GROUP = C_HEADS // C_KV_HEADS
C_Q = C_HEADS * HEAD_DIM
C_KV = C_KV_HEADS * HEAD_DIM
C_WINDOW = 128

N_EXPERTS = 32
TOP_K = 4
F_EXPERT = 192
SWIGLU_LIMIT = 7.0
SWIGLU_ALPHA = 1.702

EVEN_SPLITS = (A_Q, A_KV, A_KV, B_D_INNER, B_CONV_CH, 2 * B_HEADS)
EVEN_IN = sum(EVEN_SPLITS)
EVEN_MIX = A_Q + B_D_INNER
ODD_SPLITS = (C_Q, C_KV, C_KV)
ODD_IN = sum(ODD_SPLITS)

kernel_name = "hybrid_dit_gqa_ssd_swa_moe"


def rms_norm(x, g):
    xf = x.astype(jnp.float32)
    y = xf * lax.rsqrt(jnp.mean(xf * xf, axis=-1, keepdims=True) + NORM_EPS)
    return (y * g.astype(jnp.float32)).astype(x.dtype)


def modulate(h, shift, scale):
    return h * (1 + scale) + shift


def ada_mod(cvec, w, b, n_chunks):
    width = n_chunks * D_MODEL
    m = jax.nn.silu(cvec) @ w[:, :width] + b[:width]
    return jnp.split(m, n_chunks, axis=-1)


def split_cols(t, sizes):
    return jnp.split(t, [int(v) for v in np.cumsum(sizes)[:-1]], axis=-1)


def to_heads(t, n_heads):
    return t.reshape(t.shape[0], t.shape[1], n_heads, -1)


def axial_rope_tables(n_tokens):
    rows = n_tokens // GRID_W
    row = jnp.repeat(jnp.arange(rows, dtype=jnp.float32), GRID_W)
    col = jnp.tile(jnp.arange(GRID_W, dtype=jnp.float32), rows)
    n_freq = HEAD_DIM // 4
    inv_freq = ROPE_THETA ** (-jnp.arange(n_freq, dtype=jnp.float32) / n_freq)
    ang = jnp.concatenate([row[:, None] * inv_freq, col[:, None] * inv_freq], axis=-1)
    return jnp.cos(ang), jnp.sin(ang)


def apply_axial_rope(x, cos, sin):
    b, n, h, d = x.shape
    xa = x.astype(jnp.float32).reshape(b, n, h, 2, 2, d // 4)
    cs = cos.reshape(n, 1, 2, d // 4)
    sn = sin.reshape(n, 1, 2, d // 4)
    x1, x2 = xa[..., 0, :], xa[..., 1, :]
    out = jnp.stack([x1 * cs - x2 * sn, x2 * cs + x1 * sn], axis=-2)
    return out.reshape(b, n, h, d).astype(x.dtype)


def attend(q, k, v, mask=None, sink=None):
    s = jnp.einsum("bqkgd,btkd->bkgqt", q, k, preferred_element_type=jnp.float32) * (q.shape[-1] ** -0.5)
    if mask is not None:
        s = jnp.where(mask, s, -jnp.inf)
    if sink is None:
        p = jax.nn.softmax(s, axis=-1)
    else:
        sk = jnp.broadcast_to(sink.astype(jnp.float32)[None, :, :, None, None], s.shape[:-1] + (1,))
        p = jax.nn.softmax(jnp.concatenate([s, sk], axis=-1), axis=-1)[..., :-1]
    o = jnp.einsum("bkgqt,btkd->bqkgd", p.astype(v.dtype), v)
    return o.reshape(o.shape[0], o.shape[1], -1)


def depthwise_conv(x, w, b):
    pad = B_CONV_W // 2
    y = lax.conv_general_dilated(x, w[:, None, :].astype(x.dtype), window_strides=(1,), padding=[(pad, pad)],
                                 dimension_numbers=("NWC", "WIO", "NWC"), feature_group_count=x.shape[-1])
    return y + b


def ssd_scan(xh, dt, a_coef, bm, cm, h0):
    f32 = jnp.float32
    b, l, g, j, p = xh.shape
    n = bm.shape[-1]
    nc = l // B_CHUNK
    ld = (dt.astype(f32) * a_coef.astype(f32)).reshape(b, nc, B_CHUNK, g, j)
    xdt = (xh.astype(f32) * dt.astype(f32)[..., None]).reshape(b, nc, B_CHUNK, g, j, p)
    bmc = bm.astype(f32).reshape(b, nc, B_CHUNK, g, n)
    cmc = cm.astype(f32).reshape(b, nc, B_CHUNK, g, n)
    cum = jnp.cumsum(ld, axis=2)
    lower = jnp.tril(jnp.ones((B_CHUNK, B_CHUNK), dtype=bool))[None, None, :, :, None, None]
    seg = jnp.exp(jnp.where(lower, cum[:, :, :, None] - cum[:, :, None, :], -jnp.inf))
    cb = jnp.einsum("bclgn,bcsgn->bclsg", cmc, bmc)
    y_diag = jnp.einsum("bclsg,bclsgj,bcsgjp->bclgjp", cb, seg, xdt)
    states = jnp.einsum("bcsgn,bcsgj,bcsgjp->bcgjpn", bmc, jnp.exp(cum[:, :, -1:] - cum), xdt)
    chunk_decay = jnp.exp(cum[:, :, -1])

    def step(h, inp):
        st, dec = inp
        return h * dec[..., None, None] + st, h

    h_last, h_prev = lax.scan(step, h0.astype(f32), (jnp.moveaxis(states, 1, 0), jnp.moveaxis(chunk_decay, 1, 0)))
    h_prev = jnp.moveaxis(h_prev, 0, 1)
    y_off = jnp.einsum("bclgn,bcgjpn,bclgj->bclgjp", cmc, h_prev, jnp.exp(cum))
    return (y_diag + y_off).reshape(b, l, g, j, p), h_last


def ssm_inputs(xbc, dt_raw, conv_w, conv_b, dt_bias):
    b, l, _ = xbc.shape
    xbc = jax.nn.silu(depthwise_conv(xbc, conv_w, conv_b))
    xs, bm, cm = split_cols(xbc, (B_D_INNER, B_GN, B_GN))
    xs = xs.reshape(b, l, B_GROUPS, B_HPG, B_HEAD_DIM)
    bm = bm.reshape(b, l, B_GROUPS, B_STATE)
    cm = cm.reshape(b, l, B_GROUPS, B_STATE)
    dt = jax.nn.softplus((dt_raw.reshape(b, l, 2, B_GROUPS, B_HPG)
                          + dt_bias.reshape(2, B_GROUPS, B_HPG)).astype(jnp.float32))
    return xs, bm, cm, dt


def gated_norm(y, z, g):
    b, l = z.shape[:2]
    yz = y.reshape(b, l, B_GROUPS, -1).astype(z.dtype) * jax.nn.silu(z.reshape(b, l, B_GROUPS, -1))
    return rms_norm(yz, g.reshape(B_GROUPS, -1)).reshape(b, l, B_D_INNER)


def maybe_flip(t, direction):
    return jnp.flip(t, axis=1) if direction == 1 else t


def ssd_mixer(zl, xbcl, dtl, zc, xbcc, dtc, conv_w, conv_b, a_log, dt_bias, d_skip, ssm_g):
    xl, bl, cl, dl = ssm_inputs(xbcl, dtl, conv_w, conv_b, dt_bias)
    xc, bc, cc, dc = ssm_inputs(xbcc, dtc, conv_w, conv_b, dt_bias)
    a_coef = -jnp.exp(a_log.astype(jnp.float32)).reshape(2, B_GROUPS, B_HPG)
    skip = d_skip.reshape(2, B_GROUPS, B_HPG, 1)
    h0 = jnp.zeros((zl.shape[0], B_GROUPS, B_HPG, B_HEAD_DIM, B_STATE), jnp.float32)
    ys_l, ys_c = [], []
    for d in range(2):
        yc, hc_last = ssd_scan(maybe_flip(xc, d), maybe_flip(dc[:, :, d], d), a_coef[d],
                               maybe_flip(bc, d), maybe_flip(cc, d), h0)
        yl, _ = ssd_scan(maybe_flip(xl, d), maybe_flip(dl[:, :, d], d), a_coef[d],
                         maybe_flip(bl, d), maybe_flip(cl, d), hc_last)
        ys_c.append(maybe_flip(yc, d) + skip[d] * xc)
        ys_l.append(maybe_flip(yl, d) + skip[d] * xl)
    return gated_norm(ys_l[0] + ys_l[1], zl, ssm_g), gated_norm(ys_c[0] + ys_c[1], zc, ssm_g)


def even_mixer(hl, hc, cos, sin, w_in, w_out, q_g, k_g, conv_w, conv_b, a_log, dt_bias, d_skip, ssm_g):
    b, s, _ = hl.shape
    n_ctx = hc.shape[1]
    ql, kl, vl, zl, xbcl, dtl = split_cols(hl @ w_in, EVEN_SPLITS)
    qc, kc, vc, zc, xbcc, dtc = split_cols(hc @ w_in, EVEN_SPLITS)

    ql = apply_axial_rope(rms_norm(to_heads(ql, A_HEADS), q_g), cos, sin)
    kl = apply_axial_rope(rms_norm(to_heads(kl, A_KV_HEADS), k_g), cos, sin)
    qc = rms_norm(to_heads(qc, A_HEADS), q_g)
    kc = rms_norm(to_heads(kc, A_KV_HEADS), k_g)
    vc = to_heads(vc, A_KV_HEADS)
    keys = jnp.concatenate([kc, kl], axis=1)
    vals = jnp.concatenate([vc, to_heads(vl, A_KV_HEADS)], axis=1)
    n_blk = s // Q_BLOCK
    q_blocks = jnp.moveaxis(ql.reshape(b, n_blk, Q_BLOCK, A_KV_HEADS, A_GROUP, HEAD_DIM), 1, 0)
    attn_l = lax.map(lambda q_blk: attend(q_blk, keys, vals), q_blocks)
    attn_l = jnp.moveaxis(attn_l, 0, 1).reshape(b, s, A_Q)
    attn_c = attend(qc.reshape(b, n_ctx, A_KV_HEADS, A_GROUP, HEAD_DIM), kc, vc)

    ssm_l, ssm_c = ssd_mixer(zl, xbcl, dtl, zc, xbcc, dtc, conv_w, conv_b, a_log, dt_bias, d_skip, ssm_g)

    out_l = jnp.concatenate([attn_l, ssm_l], axis=-1) @ w_out
    out_c = jnp.concatenate([attn_c, ssm_c], axis=-1) @ w_out
    return out_l, out_c


def odd_mixer(hl, hc, cos, sin, w_in, w_out, sinks, ctx_out):
    b, s, _ = hl.shape
    n_ctx = hc.shape[1]
    ql, kl, vl = split_cols(hl @ w_in, ODD_SPLITS)
    ql = apply_axial_rope(to_heads(ql, C_HEADS), cos, sin)
    kl = apply_axial_rope(to_heads(kl, C_KV_HEADS), cos, sin)
    vl = to_heads(vl, C_KV_HEADS)
    if ctx_out:
        qc, kc, vc = split_cols(hc @ w_in, ODD_SPLITS)
    else:
        kc, vc = split_cols(hc @ w_in[:, C_Q:], ODD_SPLITS[1:])
    kc, vc = to_heads(kc, C_KV_HEADS), to_heads(vc, C_KV_HEADS)
    sink = sinks.reshape(C_KV_HEADS, C_GROUP)
    band = Q_BLOCK + 2 * C_WINDOW
    pad = ((0, 0), (C_WINDOW, C_WINDOW), (0, 0), (0, 0))
    k_pad, v_pad = jnp.pad(kl, pad), jnp.pad(vl, pad)
    n_blk = s // Q_BLOCK
    q_blocks = jnp.moveaxis(ql.reshape(b, n_blk, Q_BLOCK, C_KV_HEADS, C_GROUP, HEAD_DIM), 1, 0)
    ctx_visible = jnp.ones((Q_BLOCK, n_ctx), dtype=bool)

    def one_block(args):
        q_blk, blk = args
        start = blk * Q_BLOCK
        k_win = lax.dynamic_slice_in_dim(k_pad, start, band, axis=1)
        v_win = lax.dynamic_slice_in_dim(v_pad, start, band, axis=1)
        key_pos = start - C_WINDOW + jnp.arange(band)
        q_pos = start + jnp.arange(Q_BLOCK)
        near = ((jnp.abs(key_pos[None, :] - q_pos[:, None]) <= C_WINDOW)
                & (key_pos[None, :] >= 0) & (key_pos[None, :] < s))
        mask = jnp.concatenate([ctx_visible, near], axis=1)
        return attend(q_blk, jnp.concatenate([kc, k_win], axis=1), jnp.concatenate([vc, v_win], axis=1),
                      mask=mask, sink=sink)

    attn_l = jnp.moveaxis(lax.map(one_block, (q_blocks, jnp.arange(n_blk))), 0, 1).reshape(b, s, C_Q)
    out_l = attn_l @ w_out
    if not ctx_out:
        return out_l, None
    attn_c = attend(qc.reshape(b, n_ctx, C_KV_HEADS, C_GROUP, HEAD_DIM), kc, vc, sink=sink)
    return out_l, attn_c @ w_out


def moe_ffn(h, router_w, router_b, w_gu, b_gu, w_down, b_down):
    logits = (h @ router_w + router_b).astype(jnp.float32)
    top_val, top_idx = lax.top_k(logits, TOP_K)
    top_w = jax.nn.softmax(top_val, axis=-1)
    gates = jnp.sum(jax.nn.one_hot(top_idx, N_EXPERTS, dtype=jnp.float32) * top_w[..., None], axis=-2)
    gates = gates.astype(h.dtype)
    gu = jnp.einsum("bnd,edf->bnef", h, w_gu) + b_gu
    glu = jnp.minimum(gu[..., 0::2], SWIGLU_LIMIT)
    lin = jnp.clip(gu[..., 1::2], -SWIGLU_LIMIT, SWIGLU_LIMIT)
    act = glu * jax.nn.sigmoid(SWIGLU_ALPHA * glu) * (lin + 1)
    return jnp.einsum("bnef,efd->bnd", act * gates[..., None], w_down) + gates @ b_down


def setup_inputs(seed: int = 0) -> dict:
    key = jax.random.key(seed)
    keys = jax.random.split(key, 32)
    counter = iter(range(32))

    def nk():
        return keys[next(counter)]

    def nrm(shape, scale):
        return scale * jax.random.normal(nk(), shape, jnp.float32)

    def gain(shape):
        return 1.0 + nrm(shape, 0.02)

    a_log = jnp.log(jax.random.uniform(nk(), (N_PAIRS, 2, B_HEADS), jnp.float32, 1.0, 16.0))
    dt = jnp.exp(jax.random.uniform(nk(), (N_PAIRS, 2, B_HEADS), jnp.float32,
                                    math.log(DT_MIN), math.log(DT_MAX)))
    return {
        "x": nrm((BATCH, SEQ, D_MODEL), 1.0),
        "c": nrm((BATCH, D_MODEL), 1.0),
        "ctx": nrm((BATCH, CTX_LEN, D_MODEL), 1.0),
        "c_ctx": nrm((D_MODEL,), 1.0),
        "ada_w": nrm((DEPTH, D_MODEL, N_MOD * D_MODEL), 0.5 * D_MODEL ** -0.5),
        "ada_b": nrm((DEPTH, N_MOD * D_MODEL), 0.01),
        "norm_mix_g": gain((DEPTH, D_MODEL)),
        "norm_ffn_g": gain((DEPTH, D_MODEL)),
        "router_w": nrm((DEPTH, D_MODEL, N_EXPERTS), D_MODEL ** -0.5),
        "router_b": nrm((DEPTH, N_EXPERTS), 0.01),
        "exp_w_gu": nrm((DEPTH, N_EXPERTS, D_MODEL, 2 * F_EXPERT), D_MODEL ** -0.5),
        "exp_b_gu": nrm((DEPTH, N_EXPERTS, 2 * F_EXPERT), 0.01),
        "exp_w_down": nrm((DEPTH, N_EXPERTS, F_EXPERT, D_MODEL), F_EXPERT ** -0.5),
        "exp_b_down": nrm((DEPTH, N_EXPERTS, D_MODEL), 0.01),
        "ev_w_in": nrm((N_PAIRS, D_MODEL, EVEN_IN), D_MODEL ** -0.5),
        "ev_w_out": nrm((N_PAIRS, EVEN_MIX, D_MODEL), EVEN_MIX ** -0.5),
        "ev_q_g": gain((N_PAIRS, HEAD_DIM)),
        "ev_k_g": gain((N_PAIRS, HEAD_DIM)),
        "ev_conv_w": nrm((N_PAIRS, B_CONV_W, B_CONV_CH), B_CONV_W ** -0.5),
        "ev_conv_b": nrm((N_PAIRS, B_CONV_CH), 0.01),
        "ev_a_log": a_log,
        "ev_dt_bias": dt + jnp.log(-jnp.expm1(-dt)),
        "ev_d_skip": 1.0 + nrm((N_PAIRS, 2, B_HEADS), 0.1),
        "ev_ssm_g": gain((N_PAIRS, B_D_INNER)),
        "od_w_in": nrm((N_PAIRS, D_MODEL, ODD_IN), D_MODEL ** -0.5),
        "od_w_out": nrm((N_PAIRS, C_Q, D_MODEL), C_Q ** -0.5),
        "od_sinks": nrm((N_PAIRS, C_HEADS), 1.0),
        "final_g": gain((D_MODEL,)),
    }


def reference(x, c, ctx, c_ctx, ada_w, ada_b, norm_mix_g, norm_ffn_g, router_w, router_b,
              exp_w_gu, exp_b_gu, exp_w_down, exp_b_down, ev_w_in, ev_w_out, ev_q_g, ev_k_g,
              ev_conv_w, ev_conv_b, ev_a_log, ev_dt_bias, ev_d_skip, ev_ssm_g,
              od_w_in, od_w_out, od_sinks, final_g):
    cos, sin = axial_rope_tables(x.shape[1])
    xl, xc = x, ctx
    for i in range(DEPTH):
        last = i == DEPTH - 1
        p = i // 2
        ml = ada_mod(c[:, None, :], ada_w[i], ada_b[i], N_MOD)
        mc = ada_mod(c_ctx, ada_w[i], ada_b[i], 2 if last else N_MOD)
        hl = modulate(rms_norm(xl, norm_mix_g[i]), ml[0], ml[1])
        hc = modulate(rms_norm(xc, norm_mix_g[i]), mc[0], mc[1])
        if i % 2 == 0:
            ol, oc = even_mixer(hl, hc, cos, sin, ev_w_in[p], ev_w_out[p], ev_q_g[p], ev_k_g[p],
                                ev_conv_w[p], ev_conv_b[p], ev_a_log[p], ev_dt_bias[p], ev_d_skip[p], ev_ssm_g[p])
        else:
            ol, oc = odd_mixer(hl, hc, cos, sin, od_w_in[p], od_w_out[p], od_sinks[p], not last)
        xl = xl + ml[2] * ol
        hl = modulate(rms_norm(xl, norm_ffn_g[i]), ml[3], ml[4])
        moe_args = (router_w[i], router_b[i], exp_w_gu[i], exp_b_gu[i], exp_w_down[i], exp_b_down[i])
        if last:
            xl = xl + ml[5] * moe_ffn(hl, *moe_args)
        else:
            xc = xc + mc[2] * oc
            hc = modulate(rms_norm(xc, norm_ffn_g[i]), mc[3], mc[4])
            n_ctx = xc.shape[1]
            y = moe_ffn(jnp.concatenate([hc, hl], axis=1), *moe_args)
            xc = xc + mc[5] * y[:, :n_ctx]
            xl = xl + ml[5] * y[:, n_ctx:]
    return rms_norm(xl, final_g)
```

```python
import numpy as np
import ml_dtypes
import concourse.bass as bass
import concourse.mybir as mybir
from concourse.bass_utils import run_bass_kernel_spmd

F32 = mybir.dt.float32
BF16 = mybir.dt.bfloat16
AF = mybir.ActivationFunctionType
ALU = mybir.AluOpType
AX = mybir.AxisListType

D = 4096
KT = 32
HD = 128
EPS = 1e-6
NEG = -30000.0
NE = 32
FE = 192
EVEN_IN = 8256
ODD_IN = 6144


class Sched:
    NDMA = 5

    def __init__(self, nc):
        self.nc = nc
        self.ops = []
        self.last_w = {}
        self.rd_c = {}
        self.rd_d = {}
        self.phase = 0
        self.last_eng = {}
        self.last_dma = {}
        self.dcnt = {}
        self.pending = {}

    def new_phase(self):
        self.phase += 1

    def barrier(self):
        deps = set(self.last_eng.values()) | set(self.last_dma.values())
        for e in ["pe", "act", "dve", "pool", "sp"]:
            self.pending.setdefault(e, set()).update(deps)

    def add(self, eng, fn, r=(), w=(), dma=False):
        def isps(k):
            return isinstance(k, str) and k.startswith("ps") and k[2:].isdigit()
        w = list(w) + [k for k in r if isps(k)]
        r = [k for k in r if not isps(k)]
        idx = len(self.ops)
        deps = set()
        for k in r:
            if k in self.last_w:
                deps.add(self.last_w[k])
        for k in w:
            if k in self.last_w:
                deps.add(self.last_w[k])
            for x in self.rd_c.get(k, {}).values():
                deps.add(x)
            for x in self.rd_d.get(k, ()):
                deps.add(x)
        for k in r:
            if dma:
                self.rd_d.setdefault(k, []).append(idx)
            else:
                self.rd_c.setdefault(k, {})[eng] = idx
        for k in w:
            self.last_w[k] = idx
            self.rd_c[k] = {}
            self.rd_d[k] = []
        if eng in self.pending:
            deps |= self.pending.pop(eng)
        deps.discard(idx)
        self.last_eng[eng] = idx
        if dma:
            n = self.dcnt.get((self.phase, eng), 0)
            self.dcnt[(self.phase, eng)] = n + 1
            self.last_dma[(self.phase, eng, n % self.NDMA)] = idx
        self.ops.append(dict(eng=eng, fn=fn, deps=deps, dma=dma, phase=self.phase))
        return idx

    def emit(self, stack):
        nc = self.nc
        ops = self.ops
        engs = ["pe", "act", "dve", "pool", "sp"]
        sig = [False] * len(ops)
        for i, op in enumerate(ops):
            if op["dma"]:
                sig[i] = True
            for d in op["deps"]:
                dop = ops[d]
                if dop["dma"]:
                    continue
                if dop["eng"] == op["eng"] and op["eng"] == "pe" and not op["dma"]:
                    continue
                sig[d] = True
        sems = {}

        def getsem(key):
            if key not in sems:
                sems[key] = stack.enter_context(nc.semaphore("s_%s" % "_".join(str(x) for x in key)))
            return sems[key]

        ccount = {}
        dcount = {}
        tok = [None] * len(ops)
        prevdma = [None] * len(ops)
        for i, op in enumerate(ops):
            ph, e = op["phase"], op["eng"]
            if op["dma"]:
                n = dcount.get((ph, e), 0)
                dcount[(ph, e)] = n + 1
                slot = n % self.NDMA
                s = getsem((ph, e, "d", slot))
                tok[i] = (s, 16 * (n // self.NDMA + 1))
                if n >= self.NDMA:
                    prevdma[i] = (s, 16 * (n // self.NDMA))
            elif sig[i]:
                n = ccount.get((ph, e), 0) + 1
                ccount[(ph, e)] = n
                tok[i] = (getsem((ph, e, "c")), n)
        streams = {e: [] for e in engs}
        for i, op in enumerate(ops):
            streams[op["eng"]].append(i)

        def run(engobj, ename):
            waited = {}
            for i in streams[ename]:
                op = ops[i]
                waits = []
                if prevdma[i] is not None:
                    waits.append(prevdma[i])
                for d in sorted(op["deps"]):
                    if tok[d] is not None:
                        waits.append(tok[d])
                for (s, v) in waits:
                    key = id(s)
                    if waited.get(key, 0) >= v:
                        continue
                    waited[key] = v
                    engobj.wait_ge(s, v)
                ins = op["fn"](engobj)
                if tok[i] is not None:
                    s, v = tok[i]
                    ins.then_inc(s, 16 if op["dma"] else 1)
            last = {}
            for i in streams[ename]:
                if ops[i]["dma"]:
                    s, v = tok[i]
                    last[id(s)] = (s, max(v, last.get(id(s), (s, 0))[1]))
            for (s, v) in last.values():
                engobj.wait_ge(s, v)

        block = stack.enter_context(nc.Block())

        @block.sync
        def _(e):
            run(e, "sp")

        @block.scalar
        def _(e):
            run(e, "act")

        @block.vector
        def _(e):
            run(e, "dve")

        @block.tensor
        def _(e):
            run(e, "pe")

        @block.gpsimd
        def _(e):
            run(e, "pool")


def make_consts(t_lat):
    i = np.arange(128)
    ident = (i[:, None] == i[None, :]).astype(np.float32)
    ones = np.ones((128, 128), np.float32)
    trif = (i[:, None] <= i[None, :]).astype(np.float32)
    trib = (i[:, None] >= i[None, :]).astype(np.float32)
    negf = np.where(i[None, :] < i[:, None], NEG, 0.0).astype(np.float32)
    negb = np.where(i[None, :] > i[:, None], NEG, 0.0).astype(np.float32)
    cf32 = np.concatenate([ident, ones, trif, trib, np.tile(negf, (1, 4)), np.tile(negb, (1, 4))], axis=1)
    partner = np.where((i % 64) < 32, i + 32, i - 32)
    perm = (i[:, None] == partner[None, :]).astype(np.float32)
    q = np.arange(512)
    negw = []
    for j in range(6):
        kpos = (j - 1) * 128 + i[:, None]
        negw.append(np.where(np.abs(kpos - q[None, :]) <= 128, 0.0, NEG).astype(np.float32))
    sel = np.zeros((128, NE * 128), np.float32)
    for e in range(NE):
        sel[e, e * 128:(e + 1) * 128] = 1.0
    cbf = np.concatenate([ident, ones, ones / 4096.0, ones / 128.0, perm] + negw + [sel], axis=1)
    rows = t_lat // 64
    row = np.repeat(np.arange(rows, dtype=np.float32), 64)
    col = np.tile(np.arange(64, dtype=np.float32), rows)
    inv = (np.float32(10000.0) ** (-np.arange(32, dtype=np.float32) / np.float32(32))).astype(np.float32)
    ang = np.concatenate([row[:, None] * inv, col[:, None] * inv], axis=-1).astype(np.float32)
    cs, sn = np.cos(ang).astype(np.float32), np.sin(ang).astype(np.float32)
    cosT = np.zeros((128, t_lat), np.float32)
    sinT = np.zeros((128, t_lat), np.float32)
    for d in range(128):
        axis, pair, f = d // 64, (d % 64) // 32, d % 32
        cosT[d] = cs[:, axis * 32 + f]
        sinT[d] = sn[:, axis * 32 + f] * (-1.0 if pair == 0 else 1.0)
    return cf32, cbf.astype(ml_dtypes.bfloat16), cosT, sinT


CF_ID, CF_ONE, CF_TRIF, CF_TRIB, CF_NEGF, CF_NEGB = 0, 128, 256, 384, 512, 1024
CF_N = 1536
CB_ID, CB_ONE, CB_ONED, CB_ONEH, CB_PERM, CB_NEGW, CB_SEL = 0, 128, 256, 384, 512, 640, 640 + 3072
CB_N = CB_SEL + 4096


def _sz(dt):
    return 4 if dt == F32 else 2


class MK:
    def __init__(self, t_lat, t_ctx, layers=(0, 1, 2, 3), final=True):
        self.nc = bass.Bass("TRN2", target_bir_lowering=False)
        self.S = Sched(self.nc)
        self.T_LAT, self.T_CTX, self.T_ALL = t_lat, t_ctx, t_lat + t_ctx
        self.layers = tuple(layers)
        self.final = final
        self.tiles = []
        for o in range(0, t_lat, 512):
            self.tiles.append(dict(ctx=False, off=o, n=min(512, t_lat - o), g=o, idx=len(self.tiles)))
        self.tiles.append(dict(ctx=True, off=0, n=t_ctx, g=t_lat, idx=len(self.tiles)))
        self.sb_off = 16384
        self.uid = 0
        self.in_names = []
        self.x_written = False

    def sb(self, name, shape, dt):
        n = 1
        for s in shape[1:]:
            n *= s
        nbytes = (n * _sz(dt) + 63) // 64 * 64
        self.uid += 1
        h = self.nc.alloc_sbuf_tensor_at("%s_%d" % (name, self.uid), list(shape), dt, offset=self.sb_off)
        self.sb_off += nbytes
        assert self.sb_off <= 229376 - 64, ("SBUF overflow", name, self.sb_off)
        return h

    def mark(self):
        return self.sb_off

    def release(self, m):
        self.S.barrier()
        self.sb_off = m

    def din(self, name, shape, dt=F32):
        self.in_names.append(name)
        return self.nc.dram_tensor(name, list(shape), dt, kind="ExternalInput").ap()

    def dscr(self, name, shape, dt):
        return self.nc.dram_tensor(name, list(shape), dt).ap()

    def pe(self, fn, r=(), w=()):
        return self.S.add("pe", fn, r, w)

    def act(self, fn, r=(), w=()):
        return self.S.add("act", fn, r, w)

    def dve(self, fn, r=(), w=()):
        return self.S.add("dve", fn, r, w)

    def pool(self, fn, r=(), w=()):
        return self.S.add("pool", fn, r, w)

    def dma(self, q, out, in_, r=(), w=()):
        return self.S.add(q, lambda e: e.dma_start(out=out, in_=in_), r, w, dma=True)

    def declare(self):
        nc = self.nc
        TL, TC, TA = self.T_LAT, self.T_CTX, self.T_ALL
        I = {}
        I["xT"] = self.din("xT", [D, TL])
        I["cxT"] = self.din("cxT", [D, TC])
        I["cT"] = self.din("cT", [128, KT, 2])
        I["cf"] = self.din("cf", [128, CF_N])
        I["cb"] = self.din("cb", [128, CB_N], BF16)
        I["cosT"] = self.din("cosT", [128, TL])
        I["sinT"] = self.din("sinT", [128, TL])
        I["final_g"] = self.din("final_g", [128, KT])
        for i in self.layers:
            p = i // 2
            I["ada_w%d" % i] = self.din("ada_w%d" % i, [D, 6 * D])
            I["ada_b%d" % i] = self.din("ada_b%d" % i, [128, 192])
            I["gmix%d" % i] = self.din("gmix%d" % i, [128, KT])
            I["gffn%d" % i] = self.din("gffn%d" % i, [128, KT])
            I["rw%d" % i] = self.din("rw%d" % i, [D, NE])
            I["rb%d" % i] = self.din("rb%d" % i, [128, NE])
            I["wgu%d" % i] = self.din("wgu%d" % i, [NE, D, 2 * FE])
            I["bgu%d" % i] = self.din("bgu%d" % i, [128, NE, 4])
            I["wdn%d" % i] = self.din("wdn%d" % i, [NE, FE, D])
            I["bdn%d" % i] = self.din("bdn%d" % i, [NE, D])
            if i % 2 == 0:
                I["win%d" % i] = self.din("win%d" % i, [D, EVEN_IN])
                I["wout%d" % i] = self.din("wout%d" % i, [D, D])
                I["qkg%d" % i] = self.din("qkg%d" % i, [128, 2])
                I["cw%d" % i] = self.din("cw%d" % i, [128, 24, 5])
                I["cbias%d" % i] = self.din("cbias%d" % i, [128, 24])
                I["alog%d" % i] = self.din("alog%d" % i, [128, 64])
                I["dtb%d" % i] = self.din("dtb%d" % i, [64, 1])
                I["dsk%d" % i] = self.din("dsk%d" % i, [128, 64])
                I["ssmg%d" % i] = self.din("ssmg%d" % i, [128, 2048])
            else:
                I["win%d" % i] = self.din("win%d" % i, [D, ODD_IN])
                I["wout%d" % i] = self.din("wout%d" % i, [D, D])
                I["sink%d" % i] = self.din("sink%d" % i, [128, 32])
        self.I = I
        self.outT = nc.dram_tensor("outT", [D, TL], F32, kind="ExternalOutput").ap()
        self.xc = self.dscr("xc_res", [D, TC], F32)
        self.qT = self.dscr("qT", [D, TA], BF16)
        self.kTd = self.dscr("kT", [1024, TA], BF16)
        self.vTM = self.dscr("vTM", [TA, 1024], BF16)
        self.mixT = self.dscr("mixT", [D, TA], BF16)
        self.xbcT = self.dscr("xbcT", [3072, TA], F32)
        self.zsT = self.dscr("zsT", [2048, TA], BF16)
        self.dtT = self.dscr("dtT", [64, TA], F32)
        self.yfw = self.dscr("yfw", [TA, 2048], F32)
        self.w_in_bf = self.dscr("w_in_bf", [D, EVEN_IN], BF16)
        self.w_out_bf = self.dscr("w_out_bf", [D, D], BF16)
        self.w_gu_bf = self.dscr("w_gu_bf", [NE, D, 2 * FE], BF16)
        self.w_dn_bf = self.dscr("w_dn_bf", [NE, FE, D], BF16)
        self.ps = [nc.alloc_psum_tensor("ps%d" % b, [128, 512], F32) for b in range(8)]
        self.cf = self.sb("cf", [128, CF_N], F32)
        self.cb = self.sb("cb", [128, CB_N], BF16)
        self.mods = {i: self.sb("mods%d" % i, [128, 192, 2], F32) for i in self.layers}
        self.gd = {i: self.sb("gd%d" % i, [128, 2, 2, KT], F32) for i in self.layers}
        self.epsc = self.sb("epsc", [128, 1], F32)
        self.dve(lambda e: e.memset(self.epsc[:, :], EPS), w=["epsc"])
        self.dma("sp", self.cf[:, :], I["cf"], w=["cf"])
        self.dma("sp", self.cb[:, :], I["cb"], w=["cb"])

    def cfv(self, off, n=128):
        return self.cf[:, off:off + n]

    def cbv(self, off, n=128):
        return self.cb[:, off:off + n]

    def x_rd(self, layer, first_of_layer0, tile):
        if first_of_layer0:
            src = self.I["cxT"] if tile["ctx"] else self.I["xT"]
        else:
            src = self.xc if tile["ctx"] else self.outT
        return src.rearrange("(kt p) t -> p kt t", p=128)[:, :, tile["off"]:tile["off"] + tile["n"]]

    def x_wr(self, tile):
        dst = self.xc if tile["ctx"] else self.outT
        return dst.rearrange("(kt p) t -> p kt t", p=128)[:, :, tile["off"]:tile["off"] + tile["n"]]

    def modv(self, i, v, chunk, kt):
        return self.mods[i][:, chunk * 32 + kt, v:v + 1]

    def ada(self):
        I = self.I
        self.gvec = {}
        for i in self.layers:
            gv = self.sb("gvec%d" % i, [128, 2, KT], F32)
            self.gvec[i] = gv
            self.dma("sp", gv[:, 0, :], I["gmix%d" % i], w=[("gvec", i, 0)])
            self.dma("sp", gv[:, 1, :], I["gffn%d" % i], w=[("gvec", i, 1)])
        self.fg = self.sb("fg", [128, KT], F32)
        self.dma("sp", self.fg[:, :], I["final_g"], w=["fg"])
        m0 = self.mark()
        cT = self.sb("cT", [128, KT, 2], F32)
        sT = self.sb("sT", [128, KT, 2], F32)
        self.dma("sp", cT[:, :, :], I["cT"], w=["cT"])
        self.act(lambda e: e.activation(out=sT[:, :, :], in_=cT[:, :, :], func=AF.Silu), r=["cT"], w=["sT"])
        slabs = [self.sb("adas%d" % b, [128, KT, 128], F32) for b in range(3)]
        biases = {i: self.sb("adab%d" % i, [128, 192], F32) for i in self.layers}
        psm = self.ps[0]
        for i in self.layers:
            W = I["ada_w%d" % i].rearrange("(kt p) m -> p kt m", p=128)
            bias = biases[i]
            self.dma("sp", bias[:, :], I["ada_b%d" % i], w=[("adab", i)])
            for ct in range(192):
                sl = slabs[ct % 3]
                key = ("adas", ct % 3)
                self.dma("sp", sl[:, :, :], W[:, :, ct * 128:(ct + 1) * 128], w=[key])

                def f(e, sl=sl, ct=ct):
                    for kt in range(KT):
                        ins = e.matmul(psm[:, ct * 2:ct * 2 + 2], sl[:, kt, :], sT[:, kt, :],
                                       start=(kt == 0), stop=(kt == KT - 1))
                    return ins
                self.pe(f, r=[key, "sT"], w=["ps0"])
            mods = self.mods[i]
            self.dve(lambda e, mods=mods, bias=bias: e.tensor_tensor(
                out=mods[:, :, :], in0=psm[:, 0:384].rearrange("p (c v) -> p c v", v=2),
                in1=bias[:, :].unsqueeze(2).to_broadcast([128, 192, 2]), op=ALU.add),
                r=["ps0", ("adab", i)], w=[("mods", i)])
            gd = self.gd[i]
            gv = self.gvec[i]
            for v in range(2):
                for wi, ch in enumerate((1, 4)):
                    self.dve(lambda e, gd=gd, mods=mods, gv=gv, v=v, wi=wi, ch=ch: e.scalar_tensor_tensor(
                        out=gd[:, v, wi, :], in0=mods[:, ch * 32:(ch + 1) * 32, v], scalar=1.0, in1=gv[:, wi, :],
                        op0=ALU.add, op1=ALU.mult),
                        r=[("mods", i), ("gvec", i, wi)], w=[("gd", i)])
        self.release(m0)

    def alloc_norm(self):
        self.xg = [self.sb("xg%d" % b, [128, 4, 512], F32) for b in range(2)]
        self.sq = [self.sb("sq%d" % b, [128, 4, 512], BF16) for b in range(2)]
        self.rstd = self.sb("rstd", [128, 512], F32)
        self.ngc = 0

    def norm_mod(self, i, which, tile, hT, hname, first0, G=None, B=None, out32=None):
        n, v, ti = tile["n"], (1 if tile["ctx"] else 0), tile["idx"]
        xv = self.x_rd(i, first0, tile)
        ps = self.ps[7]
        onesD = self.cbv(CB_ONED)
        rstd = self.rstd
        for g in range(8):
            b = self.ngc % 2
            self.ngc += 1
            xg, sq = self.xg[b], self.sq[b]
            kx, ks = ("xg", b), ("sq", b)
            self.dma("sp", xg[:, :, :n], xv[:, g * 4:(g + 1) * 4, :], r=[("x", ti, kt) for kt in range(g * 4, g * 4 + 4)], w=[kx])
            self.act(lambda e, xg=xg, sq=sq: e.activation(out=sq[:, :, :n], in_=xg[:, :, :n], func=AF.Square), r=[kx], w=[ks])

            def f(e, sq=sq, g=g):
                for k in range(4):
                    ins = e.matmul(ps[:, :n], onesD, sq[:, k, :n], start=(g == 0 and k == 0), stop=(g == 7 and k == 3))
                return ins
            self.pe(f, r=[ks, "cb"], w=["ps7"])
        self.act(lambda e: e.activation(out=rstd[:, :n], in_=ps[:, :n], func=AF.Sqrt, bias=self.epsc[:, 0:1], scale=1.0), r=["ps7", "epsc"], w=["rstd"])
        self.dve(lambda e: e.reciprocal(out=rstd[:, :n], in_=rstd[:, :n]), r=["rstd"], w=["rstd"])
        for g in range(8):
            b = self.ngc % 2
            self.ngc += 1
            xg = self.xg[b]
            kx = ("xg", b)
            self.dma("sp", xg[:, :, :n], xv[:, g * 4:(g + 1) * 4, :], r=[("x", ti, kt) for kt in range(g * 4, g * 4 + 4)], w=[kx])
            self.dve(lambda e, xg=xg: e.tensor_tensor(out=xg[:, :, :n], in0=xg[:, :, :n],
                                                      in1=rstd[:, :n].unsqueeze(1).to_broadcast([128, 4, n]), op=ALU.mult),
                     r=[kx, "rstd"], w=[kx])
            for k in range(4):
                kt = g * 4 + k
                if G is None:
                    Gs = self.gd[i][:, v, which, kt:kt + 1]
                    Bs = self.modv(i, v, 0 if which == 0 else 3, kt)
                    rk = [kx, ("gd", i), ("mods", i)]
                else:
                    Gs, Bs, rk = G[:, kt:kt + 1], 0.0, [kx, "fg"]
                if out32 is None:
                    self.act(lambda e, xg=xg, k=k, kt=kt, Gs=Gs, Bs=Bs: e.activation(
                        out=hT[:, kt, :n], in_=xg[:, k, :n], func=AF.Identity, scale=Gs, bias=Bs),
                        r=rk, w=[(hname, kt)])
                else:
                    self.act(lambda e, xg=xg, k=k, kt=kt, Gs=Gs, Bs=Bs: e.activation(
                        out=xg[:, k, :n], in_=xg[:, k, :n], func=AF.Identity, scale=Gs, bias=Bs),
                        r=rk, w=[kx])
            if out32 is not None:
                self.dma("sp", out32[:, g * 4:(g + 1) * 4, :], xg[:, :, :n], r=[kx], w=[("x", ti, kt) for kt in range(g * 4, g * 4 + 4)])

    def alloc_lin(self):
        self.wb = [self.sb("wb%d" % b, [128, KT, 128], BF16) for b in range(3)]
        self.wcnt = 0
        self.pcnt = 0

    def linear(self, hT, hkeys, n, Wv, M, epi, mts=None):
        nmt = (M + 127) // 128
        for mt in (range(nmt) if mts is None else mts):
            m0 = mt * 128
            msz = min(128, M - m0)
            b = self.wcnt % 3
            self.wcnt += 1
            wb, wkey = self.wb[b], ("wb", b)
            self.dma("pool", wb[:, :, :msz], Wv[:, :, m0:m0 + msz], w=[wkey])
            pb = self.pcnt % 2
            self.pcnt += 1
            ps, pkey = self.ps[pb], "ps%d" % pb

            def f(e, wb=wb, ps=ps, msz=msz):
                for kt in range(KT):
                    ins = e.matmul(ps[:msz, :n], wb[:, kt, :msz], hT[:, kt, :n], start=(kt == 0), stop=(kt == KT - 1))
                return ins
            self.pe(f, r=[wkey] + hkeys, w=[pkey])
            if getattr(self, "dbg", 0) == 2:
                continue
            epi(mt, msz, ps, pkey)

    def alloc_res(self):
        self.xt = [self.sb("xt%d" % b, [128, 512], F32) for b in range(3)]
        self.xtc = 0

    def res_epi(self, i, tile, gate_chunk, first0):
        n, v, ti = tile["n"], (1 if tile["ctx"] else 0), tile["idx"]
        xrd = self.x_rd(i, first0, tile)
        xwr = self.x_wr(tile)

        def epi(mt, msz, ps, pkey):
            b = self.xtc % 3
            self.xtc += 1
            xt, xk = self.xt[b], ("xt", b)
            gate = self.modv(i, v, gate_chunk, mt)
            self.dma("sp", xt[:, :n], xrd[:, mt, :], r=[("x", ti, mt)], w=[xk])
            self.dve(lambda e: e.scalar_tensor_tensor(out=xt[:, :n], in0=ps[:, :n], scalar=gate, in1=xt[:, :n],
                                                      op0=ALU.mult, op1=ALU.add), r=[pkey, xk, ("mods", i)], w=[xk])
            self.dma("sp", xwr[:, mt, :], xt[:, :n], r=[xk], w=[("x", ti, mt)])
        return epi

    def moe(self, i, last):
        I = self.I
        m0 = self.mark()
        self.xg = [self.sb("xg%d" % b, [128, 4, 512], F32) for b in range(2)]
        self.sq = [self.sb("sq%d" % b, [128, 4, 512], BF16) for b in range(2)]
        self.rstd = self.sb("rstd", [128, 512], F32)
        self.ngc = 0
        self.alloc_lin()
        self.alloc_res()
        hT = self.sb("hT", [128, KT, 512], BF16)
        actT = self.sb("actT", [128, NE, 2, 512], BF16)
        rw = self.sb("rw", [128, KT, NE], BF16)
        rb = self.sb("rb", [128, NE], F32)
        bgu = self.sb("bgu", [128, NE, 4], F32)
        bdn = self.sb("bdn", [NE, D], BF16)
        lg = self.sb("lg", [128, NE], F32)
        mx = self.sb("mx", [128, 8], F32)
        negm = self.sb("negm", [128, 1], F32)
        ex = self.sb("ex", [128, NE], F32)
        msk = self.sb("msk", [128, NE], F32)
        ssum = self.sb("ssum", [128, 1], F32)
        gts = self.sb("gts", [128, 4, NE], BF16)
        gT = self.sb("gT", [NE, 512], BF16)
        tg = [self.sb("tg%d" % b, [128, 512], F32) for b in range(2)]
        tsg = [self.sb("tsg%d" % b, [128, 512], F32) for b in range(2)]
        tl = [self.sb("tl%d" % b, [128, 512], F32) for b in range(2)]
        self.dma("pool", rw[:, :, :], I["rw%d" % i].rearrange("(kt p) e -> p kt e", p=128), w=["rw"])
        self.dma("sp", rb[:, :], I["rb%d" % i], w=["rb"])
        self.dma("sp", bgu[:, :, :], I["bgu%d" % i], w=["bgu"])
        self.dma("pool", bdn[:, :], I["bdn%d" % i], w=["bdn"])
        self.dve(lambda e: e.tensor_scalar(out=bgu[:, :, 1:2], in0=bgu[:, :, 1:2], scalar1=1.0, scalar2=None, op0=ALU.add), r=["bgu"], w=["bgu"])
        self.dve(lambda e: e.tensor_scalar(out=bgu[:, :, 3:4], in0=bgu[:, :, 3:4], scalar1=1.0, scalar2=None, op0=ALU.add), r=["bgu"], w=["bgu"])
        Wgu = self.w_gu_bf
        Wdn = self.w_dn_bf.rearrange("e f d -> f e d")
        ident = self.cbv(CB_ID)
        psb = self.ps[7][:, :].bitcast(BF16)
        hkeys = [("hT", kt) for kt in range(KT)]
        ecs = {"c": 0}

        def do_tile(tile):
            if last and tile["ctx"]:
                return
            n = tile["n"]
            self.norm_mod(i, 1, tile, hT, "hT", False)
            psr = self.ps[6]
            for sub in range(n // 128):
                def f(e, sub=sub):
                    for kt in range(KT):
                        ins = e.matmul(psr[:, sub * 32:(sub + 1) * 32], hT[:, kt, sub * 128:(sub + 1) * 128], rw[:, kt, :],
                                       start=(kt == 0), stop=(kt == KT - 1))
                    return ins
                self.pe(f, r=hkeys + ["rw"], w=["ps6"])
                self.dve(lambda e, sub=sub: e.tensor_tensor(out=lg[:, :], in0=psr[:, sub * 32:(sub + 1) * 32], in1=rb[:, :], op=ALU.add),
                         r=["ps6", "rb"], w=["lg"])
                self.dve(lambda e: e.max(out=mx[:, :], in_=lg[:, :]), r=["lg"], w=["mx"])
                self.dve(lambda e: e.tensor_scalar(out=negm[:, :], in0=mx[:, 0:1], scalar1=-1.0, scalar2=None, op0=ALU.mult), r=["mx"], w=["negm"])
                self.act(lambda e: e.activation(out=ex[:, :], in_=lg[:, :], func=AF.Exp, bias=negm[:, 0:1], scale=1.0), r=["lg", "negm"], w=["ex"])
                self.dve(lambda e: e.tensor_scalar(out=msk[:, :], in0=lg[:, :], scalar1=mx[:, 3:4], scalar2=None, op0=ALU.is_ge), r=["lg", "mx"], w=["msk"])
                self.dve(lambda e: e.tensor_tensor(out=ex[:, :], in0=ex[:, :], in1=msk[:, :], op=ALU.mult), r=["ex", "msk"], w=["ex"])
                self.dve(lambda e: e.reduce_sum(out=ssum[:, :], in_=ex[:, :], axis=AX.X), r=["ex"], w=["ssum"])
                self.dve(lambda e: e.reciprocal(out=ssum[:, :], in_=ssum[:, :]), r=["ssum"], w=["ssum"])
                self.dve(lambda e, sub=sub: e.tensor_scalar(out=gts[:, sub, :], in0=ex[:, :], scalar1=ssum[:, 0:1], scalar2=None, op0=ALU.mult),
                         r=["ex", "ssum"], w=[("gts", sub)])
                self.pe(lambda e, sub=sub: e.transpose(out=psb[0:NE, sub * 128:(sub + 1) * 128], in_=gts[:, sub, :], identity=ident),
                        r=[("gts", sub), "cb"], w=["ps7"])
            self.dve(lambda e: e.tensor_copy(out=gT[:, :n], in_=psb[0:NE, :n]), r=["ps7"], w=["gT"])
            for ex_i in range(NE):
                Wv = Wgu[ex_i].rearrange("(kt p) m -> p kt m", p=128)
                gb = 4 + (ecs["c"] % 2)
                ecs["c"] += 1
                psg, gkey = self.ps[gb], "ps%d" % gb
                sel = self.cb[0:NE, CB_SEL + ex_i * 128:CB_SEL + (ex_i + 1) * 128]
                self.pe(lambda e, psg=psg, sel=sel: e.matmul(psg[:, :n], sel, gT[:, :n], start=True, stop=True), r=["gT", "cb"], w=[gkey])
                for part, (cg, cl, pp, bg_i, bl_i, pbg, pbl) in enumerate(((0, 192, 128, 0, 1, 0, 1), (128, 320, 64, 2, 3, 2, 3))):
                    pss = []
                    for (c0, pb) in ((cg, pbg), (cl, pbl)):
                        b = self.wcnt % 3
                        self.wcnt += 1
                        wb, wkey = self.wb[b], ("wb", b)
                        self.dma("pool", wb[:, :, :pp], Wv[:, :, c0:c0 + pp], w=[wkey])
                        ps, pkey = self.ps[pb], "ps%d" % pb

                        def f(e, wb=wb, ps=ps, pp=pp):
                            for kt in range(KT):
                                ins = e.matmul(ps[:pp, :n], wb[:, kt, :pp], hT[:, kt, :n], start=(kt == 0), stop=(kt == KT - 1))
                            return ins
                        self.pe(f, r=[wkey] + hkeys, w=[pkey])
                        pss.append((ps, pkey))
                    (pg, kg), (pl, kl) = pss
                    t1, t2, t3 = tg[part], tsg[part], tl[part]
                    k1, k2, k3 = ("tg", part), ("tsg", part), ("tl", part)
                    self.dve(lambda e, pg=pg, t1=t1, pp=pp, bg_i=bg_i, ex_i=ex_i: e.tensor_scalar(
                        out=t1[:pp, :n], in0=pg[:pp, :n], scalar1=bgu[:pp, ex_i, bg_i:bg_i + 1], scalar2=7.0, op0=ALU.add, op1=ALU.min),
                        r=[kg, "bgu"], w=[k1])
                    self.act(lambda e, t1=t1, t2=t2, pp=pp: e.activation(out=t2[:pp, :n], in_=t1[:pp, :n], func=AF.Sigmoid, scale=1.702),
                             r=[k1], w=[k2])
                    self.dve(lambda e, pl=pl, t3=t3, pp=pp, bl_i=bl_i, ex_i=ex_i: e.tensor_scalar(
                        out=t3[:pp, :n], in0=pl[:pp, :n], scalar1=bgu[:pp, ex_i, bl_i:bl_i + 1], scalar2=8.0, op0=ALU.add, op1=ALU.min),
                        r=[kl, "bgu"], w=[k3])
                    self.dve(lambda e, t1=t1, t2=t2, pp=pp: e.tensor_tensor(out=t1[:pp, :n], in0=t1[:pp, :n], in1=t2[:pp, :n], op=ALU.mult),
                             r=[k1, k2], w=[k1])
                    self.dve(lambda e, t1=t1, t3=t3, pp=pp: e.scalar_tensor_tensor(out=t3[:pp, :n], in0=t3[:pp, :n], scalar=-6.0, in1=t1[:pp, :n],
                                                                                 op0=ALU.max, op1=ALU.mult), r=[k1, k3], w=[k3])
                    self.dve(lambda e, t3=t3, pp=pp, psg=psg, part=part, ex_i=ex_i: e.tensor_tensor(
                        out=actT[:pp, ex_i, part, :n], in0=t3[:pp, :n], in1=psg[:pp, :n], op=ALU.mult),
                        r=[k3, gkey], w=[("actT", ex_i, part)])
            epi = self.res_epi(i, tile, 5, False)
            akeys = [("actT", e_, p_) for e_ in range(NE) for p_ in range(2)]
            for dt in range(KT):
                b = self.wcnt % 3
                self.wcnt += 1
                wa, wka = self.wb[b], ("wb", b)
                self.dma("pool", wa[:, :, :], Wdn[0:128, :, dt * 128:(dt + 1) * 128], w=[wka])
                b = self.wcnt % 3
                self.wcnt += 1
                wbb, wkb = self.wb[b], ("wb", b)
                self.dma("pool", wbb[0:64, :, :], Wdn[128:192, :, dt * 128:(dt + 1) * 128], w=[wkb])
                pb = self.pcnt % 2
                self.pcnt += 1
                ps, pkey = self.ps[pb], "ps%d" % pb

                def f(e, wa=wa, wbb=wbb, ps=ps, dt=dt):
                    for e_ in range(NE):
                        e.matmul(ps[:, :n], wa[:, e_, :], actT[:, e_, 0, :n], start=(e_ == 0), stop=False)
                    for e_ in range(NE):
                        e.matmul(ps[:, :n], wbb[0:64, e_, :], actT[0:64, e_, 1, :n], start=False, stop=False)
                    return e.matmul(ps[:, :n], bdn[:, dt * 128:(dt + 1) * 128], gT[:, :n], start=False, stop=True)
                self.pe(f, r=[wka, wkb, "bdn", "gT"] + akeys, w=[pkey])
                epi(dt, 128, ps, pkey)
        for tile in self.tiles:
            do_tile(tile)
        self.release(m0)

    def proj(self, i, ctx_q):
        even = (i % 2 == 0)
        I = self.I
        m0 = self.mark()
        self.alloc_norm()
        self.alloc_lin()
        hT = self.sb("hT", [128, KT, 512], BF16)
        cosb = self.sb("cosb", [128, 512], F32)
        sinb = self.sb("sinb", [128, 512], F32)
        qn = [self.sb("qn%d" % b, [128, 512], BF16) for b in range(2)]
        qf = [self.sb("qf%d" % b, [128, 512], F32) for b in range(2)]
        t1 = [self.sb("t1%d" % b, [128, 512], F32) for b in range(2)]
        t2 = [self.sb("t2%d" % b, [128, 512], F32) for b in range(2)]
        ob = [self.sb("ob%d" % b, [128, 512], BF16) for b in range(3)]
        o32 = [self.sb("o32%d" % b, [128, 512], F32) for b in range(2)]
        vtm = [self.sb("vtm%d" % b, [128, 4, 128], BF16) for b in range(2)]
        Wv = self.w_in_bf.rearrange("(kt p) m -> p kt m", p=128)
        ident = self.cbv(CB_ID)
        perm = self.cbv(CB_PERM)
        onesH = self.cbv(CB_ONEH)
        psb = self.ps[6][:, :].bitcast(BF16)
        SC = float(HD) ** -0.5
        if even:
            qkg = self.sb("qkg", [128, 2], F32)
            dtb = self.sb("dtb", [64, 1], F32)
            self.dma("sp", qkg[:, :], I["qkg%d" % i], w=["qkg"])
            self.dma("sp", dtb[:, :], I["dtb%d" % i], w=["dtb"])
            self.dve(lambda e: e.tensor_scalar(out=qkg[:, 0:1], in0=qkg[:, 0:1], scalar1=SC, scalar2=None, op0=ALU.mult), r=["qkg"], w=["qkg"])
            nq, nk = 16, 4
        else:
            nq, nk = 32, 8
        cnt = {"e": 0}
        hkeys = [("hT", kt) for kt in range(KT)]
        def do_tile(tile):
            n, g, isctx = tile["n"], tile["g"], tile["ctx"]
            self.norm_mod(i, 0, tile, hT, "hT", i == self.layers[0])
            if getattr(self, "dbg", 0) == 1:
                return
            if not isctx:
                self.dma("sp", cosb[:, :n], I["cosT"][:, tile["off"]:tile["off"] + n], w=["cosb"])
                self.dma("sp", sinb[:, :n], I["sinT"][:, tile["off"]:tile["off"] + n], w=["sinb"])

            def qk_epi(ps, pkey, row0, dst, sc, gcol):
                c = cnt["e"]
                cnt["e"] += 1
                b2, b3 = c % 2, c % 3
                src, skey, scl = ps, pkey, sc
                if even:
                    self.act(lambda e: e.activation(out=qn[b2][:, :n], in_=ps[:, :n], func=AF.Square), r=[pkey], w=[("qn", b2)])
                    pss, pssk = self.ps[4 + b2], "ps%d" % (4 + b2)
                    self.pe(lambda e: e.matmul(pss[:, :n], onesH, qn[b2][:, :n], start=True, stop=True), r=[("qn", b2), "cb"], w=[pssk])
                    self.act(lambda e: e.activation(out=t2[b2][:, :n], in_=pss[:, :n], func=AF.Sqrt, bias=self.epsc[:, 0:1], scale=1.0),
                             r=[pssk, "epsc"], w=[("t2", b2)])
                    self.dve(lambda e: e.reciprocal(out=t2[b2][:, :n], in_=t2[b2][:, :n]), r=[("t2", b2)], w=[("t2", b2)])
                    self.dve(lambda e: e.scalar_tensor_tensor(out=qf[b2][:, :n], in0=ps[:, :n], scalar=qkg[:, gcol:gcol + 1], in1=t2[b2][:, :n],
                                                              op0=ALU.mult, op1=ALU.mult), r=[pkey, ("t2", b2), "qkg"], w=[("qf", b2)])
                    src, skey, scl = qf[b2], ("qf", b2), 1.0
                dbg = getattr(self, "dbg", 0)
                if dbg == 5:
                    return
                if isctx or dbg == 6:
                    self.act(lambda e: e.activation(out=ob[b3][:, :n], in_=src[:, :n], func=AF.Copy, scale=scl), r=[skey], w=[("ob", b3)])
                    if dbg == 7:
                        return
                else:
                    self.act(lambda e: e.activation(out=qn[b2][:, :n], in_=src[:, :n], func=AF.Copy, scale=scl), r=[skey], w=[("qn", b2)])
                    psr, psrk = self.ps[2 + b2], "ps%d" % (2 + b2)
                    self.pe(lambda e: e.matmul(psr[:, :n], perm, qn[b2][:, :n], start=True, stop=True), r=[("qn", b2), "cb"], w=[psrk])
                    self.dve(lambda e: e.scalar_tensor_tensor(out=t1[b2][:, :n], in0=src[:, :n], scalar=scl, in1=cosb[:, :n],
                                                              op0=ALU.mult, op1=ALU.mult), r=[skey, "cosb"], w=[("t1", b2)])
                    self.dve(lambda e: e.tensor_tensor(out=t2[b2][:, :n], in0=psr[:, :n], in1=sinb[:, :n], op=ALU.mult), r=[psrk, "sinb"], w=[("t2", b2)])
                    self.dve(lambda e: e.tensor_tensor(out=ob[b3][:, :n], in0=t1[b2][:, :n], in1=t2[b2][:, :n], op=ALU.add),
                             r=[("t1", b2), ("t2", b2)], w=[("ob", b3)])
                self.dma("sp", dst[row0:row0 + 128, g:g + n], ob[b3][:, :n], r=[("ob", b3)])

            def v_epi(ps, pkey, kvh):
                c = cnt["e"]
                cnt["e"] += 1
                b2 = c % 2
                self.act(lambda e: e.activation(out=qn[b2][:, :n], in_=ps[:, :n], func=AF.Copy), r=[pkey], w=[("qn", b2)])

                def f(e):
                    for cc in range(n // 128):
                        ins = e.transpose(out=psb[:, cc * 128:(cc + 1) * 128], in_=qn[b2][:, cc * 128:(cc + 1) * 128], identity=ident)
                    return ins
                self.pe(f, r=[("qn", b2), "cb"], w=["ps6"])
                self.dve(lambda e: e.tensor_copy(out=vtm[b2][:, :n // 128, :], in_=psb[:, :n].rearrange("p (c d) -> p c d", d=128)),
                         r=["ps6"], w=[("vtm", b2)])
                self.dma("sp", self.vTM[g:g + n, kvh * 128:(kvh + 1) * 128].rearrange("(c p) d -> p c d", p=128), vtm[b2][:, :n // 128, :],
                         r=[("vtm", b2)])

            def epi(mt, msz, ps, pkey):
                dbg = getattr(self, "dbg", 0)
                if dbg in (3, 5, 6, 7) and mt >= nq + nk:
                    return
                if dbg == 4 and mt < nq + nk:
                    return
                if mt < nq:
                    qk_epi(ps, pkey, mt * 128, self.qT, SC if not even else 1.0, 0)
                elif mt < nq + nk:
                    qk_epi(ps, pkey, (mt - nq) * 128, self.kTd, 1.0, 1)
                elif mt < nq + 2 * nk:
                    v_epi(ps, pkey, mt - nq - nk)
                elif mt < 24 + 16:
                    c = cnt["e"]
                    cnt["e"] += 1
                    b3 = c % 3
                    r0 = (mt - 24) * 128
                    self.act(lambda e: e.activation(out=ob[b3][:, :n], in_=ps[:, :n], func=AF.Silu), r=[pkey], w=[("ob", b3)])
                    self.dma("sp", self.zsT[r0:r0 + 128, g:g + n], ob[b3][:, :n], r=[("ob", b3)])
                elif mt < 64:
                    c = cnt["e"]
                    cnt["e"] += 1
                    b2 = c % 2
                    r0 = (mt - 40) * 128
                    self.act(lambda e: e.activation(out=o32[b2][:, :n], in_=ps[:, :n], func=AF.Copy), r=[pkey], w=[("o32", b2)])
                    self.dma("sp", self.xbcT[r0:r0 + 128, g:g + n], o32[b2][:, :n], r=[("o32", b2)])
                else:
                    c = cnt["e"]
                    cnt["e"] += 1
                    b2 = c % 2
                    self.act(lambda e: e.activation(out=o32[b2][:64, :n], in_=ps[:64, :n], func=AF.Exp, bias=dtb[:, 0:1], scale=1.0),
                             r=[pkey, "dtb"], w=[("o32", b2)])
                    self.act(lambda e: e.activation(out=o32[b2][:64, :n], in_=o32[b2][:64, :n], func=AF.Ln, bias=1.0, scale=1.0),
                             r=[("o32", b2)], w=[("o32", b2)])
                    self.dma("sp", self.dtT[0:64, g:g + n], o32[b2][:64, :n], r=[("o32", b2)])

            M = EVEN_IN if even else ODD_IN
            nmt = (M + 127) // 128
            mts = [mt for mt in range(nmt) if not (isctx and (not ctx_q) and mt < nq)]
            self.linear(hT, hkeys, n, Wv, M, epi, mts)
        for tile in self.tiles:
            do_tile(tile)
        self.release(m0)

    def attn(self, i, ctx_q):
        even = (i % 2 == 0)
        I = self.I
        m0 = self.mark()
        TA, TL = self.T_ALL, self.T_LAT
        NCH = TA // 128
        nh = 16 if even else 32
        Ks = [self.sb("Ks%d" % b, [128, TA], BF16) for b in range(2)]
        Vs = [self.sb("Vs%d" % b, [128, NCH, 128], BF16) for b in range(2)]
        qs = [self.sb("qs%d" % b, [128, 512], BF16) for b in range(3)]
        pbuf = [self.sb("pb%d" % b, [128, 512], BF16) for b in range(3)]
        rl = [self.sb("rl%d" % b, [128, 512], F32) for b in range(2)]
        ob = [self.sb("aob%d" % b, [128, 512], BF16) for b in range(2)]
        ident = self.cbv(CB_ID)
        ones = self.cbv(CB_ONE)
        if not even:
            esink = self.sb("esink", [128, 32], F32)
            self.dma("sp", esink[:, :], I["sink%d" % i], w=["esink"])
            self.act(lambda e: e.activation(out=esink[:, :], in_=esink[:, :], func=AF.Exp), r=["esink"], w=["esink"])
        ctx_ch = [TL // 128 + c for c in range(self.T_CTX // 128)]
        st = {"it": 0, "pc": 0}

        def do_tile(kvh, kb, K, V, h, tile):
            if True:
                if True:
                    pc = st["pc"]
                    it = st["it"]
                    n, g = tile["n"], tile["g"]
                    if tile["ctx"]:
                        keys = [(c, None) for c in ctx_ch]
                    elif even:
                        keys = [(c, None) for c in range(NCH)]
                    else:
                        keys = [(c, None) for c in ctx_ch]
                        for j in range(6):
                            ks = tile["off"] + (j - 1) * 128
                            if 0 <= ks < TL:
                                keys.append((ks // 128, j))
                    qb = it % 3
                    ab = it % 2
                    st["it"] += 1
                    q = qs[qb]
                    self.dma("sp", q[:, :n], self.qT[h * 128:(h + 1) * 128, g:g + n], w=[("qs", qb)])
                    pso, psok = self.ps[2 + ab], "ps%d" % (2 + ab)
                    psl, pslk = self.ps[4 + ab], "ps%d" % (4 + ab)
                    nk = len(keys)

                    def emit_s(k):
                        c, j = keys[k]
                        sbk = (pc + k) % 2
                        pss, pssk = self.ps[sbk], "ps%d" % sbk

                        def f(e, c=c, j=j, pss=pss):
                            ins = e.matmul(pss[:, :n], K[:, c * 128:(c + 1) * 128], q[:, :n], start=True, stop=(j is None))
                            if j is not None:
                                negw = self.cb[:, CB_NEGW + j * 512:CB_NEGW + j * 512 + n]
                                ins = e.matmul(pss[:, :n], ident, negw, start=False, stop=True)
                            return ins
                        self.pe(f, r=[("K", kb), ("qs", qb), "cb"], w=[pssk])
                        pbi = (pc + k) % 3
                        self.act(lambda e, pss=pss, pbi=pbi: e.activation(out=pbuf[pbi][:, :n], in_=pss[:, :n], func=AF.Exp),
                                 r=[pssk], w=[("pb", pbi)])

                    def emit_ol(k):
                        c, j = keys[k]
                        pbi = (pc + k) % 3

                        def f(e, c=c, pbi=pbi, k=k):
                            e.matmul(pso[:, :n], V[:, c, :], pbuf[pbi][:, :n], start=(k == 0), stop=(k == nk - 1))
                            return e.matmul(psl[:, :n], ones, pbuf[pbi][:, :n], start=(k == 0), stop=(k == nk - 1))
                        self.pe(f, r=[("V", kb, (c // 8) * 8), ("pb", pbi), "cb"], w=[psok, pslk])
                    emit_s(0)
                    for k in range(nk):
                        if k + 1 < nk:
                            emit_s(k + 1)
                        emit_ol(k)
                    st["pc"] += nk
                    if even:
                        self.dve(lambda e, psl=psl, ab=ab: e.reciprocal(out=rl[ab][:, :n], in_=psl[:, :n]), r=[pslk], w=[("rl", ab)])
                    else:
                        self.dve(lambda e, psl=psl, ab=ab, h=h: e.tensor_scalar(out=rl[ab][:, :n], in0=psl[:, :n], scalar1=esink[:, h:h + 1], scalar2=None,
                                                                               op0=ALU.add), r=[pslk, "esink"], w=[("rl", ab)])
                        self.dve(lambda e, ab=ab: e.reciprocal(out=rl[ab][:, :n], in_=rl[ab][:, :n]), r=[("rl", ab)], w=[("rl", ab)])
                    self.dve(lambda e, pso=pso, ab=ab: e.tensor_tensor(out=ob[ab][:, :n], in0=pso[:, :n], in1=rl[ab][:, :n], op=ALU.mult),
                             r=[psok, ("rl", ab)], w=[("aob", ab)])
                    self.dma("sp", self.mixT[h * 128:(h + 1) * 128, g:g + n], ob[ab][:, :n], r=[("aob", ab)])
        for kvh in range(nh // 4):
            kb = kvh % 2
            K, V = Ks[kb], Vs[kb]
            self.dma("sp", K[:, :], self.kTd[kvh * 128:(kvh + 1) * 128, :], w=[("K", kb)])
            vsrc = self.vTM[:, kvh * 128:(kvh + 1) * 128].rearrange("(c p) d -> p c d", p=128)
            for c0 in range(0, NCH, 8):
                c1 = min(NCH, c0 + 8)
                self.dma("sp", V[:, c0:c1, :], vsrc[:, c0:c1, :], w=[("V", kb, c0)])
            for hh in range(4):
                h = kvh * 4 + hh
                for tile in self.tiles:
                    if tile["ctx"] and not ctx_q:
                        continue
                    do_tile(kvh, kb, K, V, h, tile)
        self.release(m0)

    def outproj(self, i, ctx_q):
        I = self.I
        m0 = self.mark()
        self.alloc_lin()
        self.alloc_res()
        hTs = [self.sb("mT%d" % b, [128, KT, 512], BF16) for b in range(2)]
        Wv = self.w_out_bf.rearrange("(kt p) m -> p kt m", p=128)
        mv = self.mixT.rearrange("(kt p) t -> p kt t", p=128)
        for ti, tile in enumerate(self.tiles):
            if tile["ctx"] and not ctx_q:
                continue
            n, g = tile["n"], tile["g"]
            hb = ti % 2
            hT = hTs[hb]
            self.dma("sp", hT[:, :, :n], mv[:, :, g:g + n], w=[("mT", hb)])
            self.linear(hT, [("mT", hb)], n, Wv, D, self.res_epi(i, tile, 2, i == self.layers[0]))
        self.release(m0)

    def precast(self, i):
        I = self.I
        m0 = self.mark()
        bufs = [self.sb("pcb%d" % b_, [128, 8256], BF16) for b_ in range(3)]
        st = {"c": 0}

        def run(srcv, dstv, P, KN, M, kstep, mstep):
            for k0 in range(0, KN, kstep):
                kk = min(kstep, KN - k0)
                for m_0 in range(0, M, mstep):
                    msz = min(mstep, M - m_0)
                    b_ = st["c"] % 3
                    st["c"] += 1
                    t = bufs[b_][:P, 0:kk * msz].rearrange("p (k m) -> p k m", m=msz)
                    self.dma("pool", t, srcv[:, k0:k0 + kk, m_0:m_0 + msz], w=[("pcb", b_)])
                    self.dma("sp", dstv[:, k0:k0 + kk, m_0:m_0 + msz], t, r=[("pcb", b_)])
        M = EVEN_IN if i % 2 == 0 else ODD_IN
        run(I["win%d" % i].rearrange("(kt p) m -> p kt m", p=128), self.w_in_bf.rearrange("(kt p) m -> p kt m", p=128)[:, :, 0:M], 128, KT, M, 4, 2064)
        run(I["wout%d" % i].rearrange("(kt p) m -> p kt m", p=128), self.w_out_bf.rearrange("(kt p) m -> p kt m", p=128), 128, KT, D, 4, 2048)
        run(I["wgu%d" % i].rearrange("e (kt p) m -> p (e kt) m", p=128), self.w_gu_bf.rearrange("e (kt p) m -> p (e kt) m", p=128), 128, NE * KT, 2 * FE, 16, 2 * FE)
        sv = I["wdn%d" % i].rearrange("e f d -> f e d")
        dv = self.w_dn_bf.rearrange("e f d -> f e d")
        run(sv[0:128], dv[0:128], 128, NE, D, 2, D)
        run(sv[128:192], dv[128:192], 64, NE, D, 2, D)
        self.release(m0)

    def build(self, stack, stages=("proj", "attn", "ssd", "outproj", "moe")):
        self.declare()
        self.ada()
        for i in self.layers:
            last = (i == 3)
            self.S.new_phase()
            self.precast(i)
            if "proj" in stages:
                self.proj(i, not last)
            if "attn" in stages:
                self.attn(i, not last)
            if i % 2 == 0 and "ssd" in stages:
                self.ssd(i)
            if "outproj" in stages:
                self.outproj(i, not last)
            if "moe" in stages:
                self.moe(i, last)
        if self.final:
            m0 = self.mark()
            self.alloc_norm()
            for tile in self.tiles:
                if not tile["ctx"]:
                    self.norm_mod(None, 0, tile, None, None, False, G=self.fg, out32=self.x_wr(tile))
            self.release(m0)
        self.S.emit(stack)
        return self.nc


def prep_inputs(inp, b, layers, t_lat, t_ctx, consts):
    cf32, cbf, cosT, sinT = consts
    f = np.float32

    def fm(vec):
        return np.ascontiguousarray(np.asarray(vec, f).reshape(KT, 128).T)

    def rep(vec):
        v = np.asarray(vec, f).reshape(1, -1)
        return np.ascontiguousarray(np.broadcast_to(v, (128, v.shape[1])))

    d = {}
    d["xT"] = np.ascontiguousarray(np.asarray(inp["x"][b, :t_lat], f).T)
    d["cxT"] = np.ascontiguousarray(np.asarray(inp["ctx"][b, :t_ctx], f).T)
    cc = np.stack([np.asarray(inp["c"][b], f), np.asarray(inp["c_ctx"], f)], axis=-1)
    d["cT"] = np.ascontiguousarray(cc.reshape(KT, 128, 2).transpose(1, 0, 2))
    d["cf"], d["cb"], d["cosT"], d["sinT"] = cf32, cbf, cosT, sinT
    d["final_g"] = fm(inp["final_g"])
    for i in layers:
        p = i // 2
        d["ada_w%d" % i] = np.asarray(inp["ada_w"][i], f)
        d["ada_b%d" % i] = np.ascontiguousarray(np.asarray(inp["ada_b"][i], f).reshape(192, 128).T)
        d["gmix%d" % i] = fm(inp["norm_mix_g"][i])
        d["gffn%d" % i] = fm(inp["norm_ffn_g"][i])
        d["rw%d" % i] = np.asarray(inp["router_w"][i], f)
        d["rb%d" % i] = rep(inp["router_b"][i])
        wgu = np.asarray(inp["exp_w_gu"][i], f)
        d["wgu%d" % i] = np.ascontiguousarray(np.concatenate([wgu[:, :, 0::2], wgu[:, :, 1::2]], axis=-1))
        bgu = np.asarray(inp["exp_b_gu"][i], f)
        bg, bl = bgu[:, 0::2], bgu[:, 1::2]
        t = np.zeros((128, NE, 4), f)
        t[:, :, 0] = bg[:, :128].T
        t[:, :, 1] = bl[:, :128].T
        t[:64, :, 2] = bg[:, 128:].T
        t[:64, :, 3] = bl[:, 128:].T
        d["bgu%d" % i] = t
        d["wdn%d" % i] = np.asarray(inp["exp_w_down"][i], f)
        d["bdn%d" % i] = np.asarray(inp["exp_b_down"][i], f)
        if i % 2 == 0:
            d["win%d" % i] = np.asarray(inp["ev_w_in"][p], f)
            d["wout%d" % i] = np.asarray(inp["ev_w_out"][p], f)
            d["qkg%d" % i] = np.ascontiguousarray(np.stack([np.asarray(inp["ev_q_g"][p], f), np.asarray(inp["ev_k_g"][p], f)], axis=-1))
            cw = np.asarray(inp["ev_conv_w"][p], f)
            d["cw%d" % i] = np.ascontiguousarray(cw.reshape(5, 24, 128).transpose(2, 1, 0))
            d["cbias%d" % i] = np.ascontiguousarray(np.asarray(inp["ev_conv_b"][p], f).reshape(24, 128).T)
            d["alog%d" % i] = rep(np.asarray(inp["ev_a_log"][p], f).reshape(-1))
            d["dtb%d" % i] = np.ascontiguousarray(np.asarray(inp["ev_dt_bias"][p], f).reshape(64, 1))
            d["dsk%d" % i] = rep(np.asarray(inp["ev_d_skip"][p], f).reshape(-1))
            d["ssmg%d" % i] = rep(inp["ev_ssm_g"][p])
        else:
            d["win%d" % i] = np.asarray(inp["od_w_in"][p], f)
            d["wout%d" % i] = np.asarray(inp["od_w_out"][p], f)
            d["sink%d" % i] = rep(inp["od_sinks"][p])
    return d


def _ssd(self, i):
    I = self.I
    m0 = self.mark()
    TL, TC = self.T_LAT, self.T_CTX
    NT = 256
    sb = self.sb
    xin = [sb("xin%d" % b, [128, 8, NT + 4], F32) for b in range(2)]
    acc = [sb("cacc%d" % b, [128, NT], F32) for b in range(2)]
    cvo = sb("cvo", [128, 24, NT], BF16)
    xTM = sb("xTM", [128, 2, 2048], BF16)
    BTM = sb("BTM", [128, 2, 512], BF16)
    zs = sb("zsF", [128, 16, NT], BF16)
    zTM = sb("zTM", [128, 2, 2048], BF16)
    dtin = sb("dtin", [64, NT], F32)
    dtTM = sb("dtTM", [128, 2, 64], F32)
    cw = sb("cw", [128, 24, 5], F32)
    cbias = sb("cbias", [128, 24], F32)
    Aneg = sb("Aneg", [128, 64], F32)
    dsk = sb("dsk", [128, 64], F32)
    sks = sb("sks", [128, 32], F32)
    ssmg = sb("ssmg", [128, 2048], F32)
    ld = sb("ld", [128, 32], F32)
    ncum = sb("ncum", [128, 32], F32)
    dec = sb("dec", [128, 32], F32)
    ecum = sb("ecum", [128, 32], F32)
    Ball = [sb("Ball%d" % b, [128, 8, 128], F32) for b in range(2)]
    rhs2 = [sb("rhs2%d" % b, [128, 8, 128], F32) for b in range(2)]
    Lall = [sb("Lall%d" % b, [128, 8, 128], BF16) for b in range(2)]
    Mall = [sb("Mall%d" % b, [128, 8, 128], BF16) for b in range(2)]
    cbs = [sb("cbs%d" % b, [128, 128], BF16) for b in range(2)]
    wv = [sb("wv%d" % b, [128, 8], F32) for b in range(2)]
    xdt = [sb("xdt%d" % b, [128, 512], BF16) for b in range(2)]
    xdw = [sb("xdw%d" % b, [128, 512], BF16) for b in range(2)]
    tmp = [sb("ytmp%d" % b, [128, 512], F32) for b in range(2)]
    yacc = [sb("yacc%d" % b, [128, 2048], F32) for b in range(2)]
    hst = sb("hst", [128, 4, 512], F32)
    hbf = sb("hbf", [128, 4, 512], BF16)
    yf = sb("yf", [128, 2048], F32)
    junk = sb("junk", [128, 512], F32)
    ssq = sb("ssq", [128, 4], F32)
    ynb = sb("ynb", [128, 2048], BF16)
    oFM = [sb("oFM%d" % b, [128, 16, 128], BF16) for b in range(2)]
    identb = self.cbv(CB_ID)
    identf = self.cfv(CF_ID)
    onesf = self.cfv(CF_ONE)
    ps = self.ps
    psbA = ps[6][:, :].bitcast(BF16)
    psbB = ps[7][:, :].bitcast(BF16)
    self.dma("sp", cw[:, :, :], I["cw%d" % i], w=["cw"])
    self.dma("sp", cbias[:, :], I["cbias%d" % i], w=["cbias"])
    self.dma("sp", Aneg[:, :], I["alog%d" % i], w=["Aneg"])
    self.dma("sp", dsk[:, :], I["dsk%d" % i], w=["dsk"])
    self.dma("sp", ssmg[:, :], I["ssmg%d" % i], w=["ssmg"])
    self.act(lambda e: e.activation(out=Aneg[:, :], in_=Aneg[:, :], func=AF.Exp), r=["Aneg"], w=["Aneg"])
    self.dve(lambda e: e.tensor_scalar(out=Aneg[:, :], in0=Aneg[:, :], scalar1=-1.0, scalar2=None, op0=ALU.mult), r=["Aneg"], w=["Aneg"])
    self.dve(lambda e: e.tensor_tensor(out=sks[:, :], in0=dsk[:, 0:32], in1=dsk[:, 32:64], op=ALU.add), r=["dsk"], w=["sks"])
    xbv = self.xbcT.rearrange("(ct p) t -> p ct t", p=128)
    zsv = self.zsT.rearrange("(ct p) t -> p ct t", p=128)
    mixv = self.mixT.rearrange("(kt p) t -> p kt t", p=128)
    cnt = {"g": 0, "c": 0}

    def do_tile(d, seq_g0, seq_len, off):
        n = min(NT, seq_len - off)
        g0 = seq_g0 + off
        nch = n // 128
        TRI = self.cfv(CF_TRIF if d == 0 else CF_TRIB)
        NEGM = self.cfv(CF_NEGF if d == 0 else CF_NEGB)
        last = 127 if d == 0 else 0
        lo, hi = max(off - 2, 0), min(off + n + 2, seq_len)
        for cg in range(3):
            b = cnt["g"] % 2
            cnt["g"] += 1
            xi, xk = xin[b], ("xin", b)
            if lo > off - 2 or hi < off + n + 2:
                self.dve(lambda e, xi=xi: e.memset(xi[:, :, :], 0.0), w=[xk])
            self.dma("sp", xi[:, :, lo - (off - 2):hi - (off - 2)], xbv[:, cg * 8:(cg + 1) * 8, seq_g0 + lo:seq_g0 + hi], w=[xk])
            for c8 in range(8):
                ct = cg * 8 + c8
                a = acc[ct % 2]
                ak = ("cacc", ct % 2)
                self.dve(lambda e, xi=xi, a=a, c8=c8, ct=ct: e.tensor_scalar(out=a[:, :n], in0=xi[:, c8, 0:n], scalar1=cw[:, ct, 0:1], scalar2=None, op0=ALU.mult),
                         r=[xk, "cw"], w=[ak])
                for k in range(1, 5):
                    self.dve(lambda e, xi=xi, a=a, c8=c8, ct=ct, k=k: e.scalar_tensor_tensor(
                        out=a[:, :n], in0=xi[:, c8, k:k + n], scalar=cw[:, ct, k:k + 1], in1=a[:, :n], op0=ALU.mult, op1=ALU.add),
                        r=[xk, "cw", ak], w=[ak])
                self.act(lambda e, a=a, ct=ct: e.activation(out=cvo[:, ct, :n], in_=a[:, :n], func=AF.Silu, bias=cbias[:, ct:ct + 1], scale=1.0),
                         r=[ak, "cbias"], w=[("cvo", ct)])
        self.dma("sp", dtin[:, :n], self.dtT[:, g0:g0 + n], w=["dtin"])
        if d == 1:
            self.dma("sp", zs[:, :, :n], zsv[:, :, g0:g0 + n], w=["zsF"])
        for c in range(nch):
            cs = slice(c * 128, (c + 1) * 128)

            def fx(e, c=c, cs=cs):
                for t_ in range(8):
                    e.transpose(out=psbA[:, t_ * 128:(t_ + 1) * 128], in_=cvo[:, t_, cs], identity=identb)
                for t_ in range(8):
                    ins = e.transpose(out=psbB[:, t_ * 128:(t_ + 1) * 128], in_=cvo[:, 8 + t_, cs], identity=identb)
                return ins
            self.pe(fx, r=[("cvo", t_) for t_ in range(16)] + ["cb"], w=["ps6", "ps7"])
            self.dve(lambda e, c=c: e.tensor_copy(out=xTM[:, c, 0:1024], in_=psbA[:, :]), r=["ps6"], w=[("xTM", c)])
            self.act(lambda e, c=c: e.activation(out=xTM[:, c, 1024:2048], in_=psbB[:, :], func=AF.Copy), r=["ps7"], w=[("xTM", c)])

            def fb(e, c=c, cs=cs):
                for t_ in range(4):
                    ins = e.transpose(out=psbA[:, t_ * 128:(t_ + 1) * 128], in_=cvo[:, 16 + t_, cs], identity=identb)
                return ins
            self.pe(fb, r=[("cvo", 16 + t_) for t_ in range(4)] + ["cb"], w=["ps6"])
            self.dve(lambda e, c=c: e.tensor_copy(out=BTM[:, c, :], in_=psbA[:, 0:512]), r=["ps6"], w=[("BTM", c)])
            self.pe(lambda e, c=c, cs=cs: e.transpose(out=ps[5][:, 0:64], in_=dtin[:, cs], identity=identf[0:64, 0:64]), r=["dtin", "cf"], w=["ps5"])
            self.dve(lambda e, c=c: e.tensor_copy(out=dtTM[:, c, :], in_=ps[5][:, 0:64]), r=["ps5"], w=[("dtTM", c)])
            if d == 1:
                def fz(e, c=c, cs=cs):
                    for t_ in range(8):
                        e.transpose(out=psbA[:, t_ * 128:(t_ + 1) * 128], in_=zs[:, t_, cs], identity=identb)
                    for t_ in range(8):
                        ins = e.transpose(out=psbB[:, t_ * 128:(t_ + 1) * 128], in_=zs[:, 8 + t_, cs], identity=identb)
                    return ins
                self.pe(fz, r=["zsF", "cb"], w=["ps6", "ps7"])
                self.dve(lambda e, c=c: e.tensor_copy(out=zTM[:, c, 0:1024], in_=psbA[:, :]), r=["ps6"], w=[("zTM", c)])
                self.act(lambda e, c=c: e.activation(out=zTM[:, c, 1024:2048], in_=psbB[:, :], func=AF.Copy), r=["ps7"], w=[("zTM", c)])
        for c in (range(nch) if d == 0 else range(nch - 1, -1, -1)):
            do_chunk(d, c, g0 + c * 128, TRI, NEGM, last)

    def do_chunk(d, c, col0, TRI, NEGM, last):
        cs = slice(c * 128, (c + 1) * 128)
        yb = cnt["c"] % 2
        cnt["c"] += 1
        ya, yk = yacc[yb], ("yacc", yb)
        self.dve(lambda e: e.tensor_tensor(out=ld[:, :], in0=dtTM[:, c, d * 32:(d + 1) * 32], in1=Aneg[:, d * 32:(d + 1) * 32], op=ALU.mult),
                 r=[("dtTM", c), "Aneg"], w=["ld"])

        def fc(e):
            e.matmul(ps[5][:, 0:32], TRI, ld[:, :], start=True, stop=True)
            return e.matmul(ps[5][:, 32:64], onesf, ld[:, :], start=True, stop=True)
        self.pe(fc, r=["ld", "cf"], w=["ps5"])
        self.dve(lambda e: e.tensor_scalar(out=ncum[:, :], in0=ps[5][:, 0:32], scalar1=-1.0, scalar2=None, op0=ALU.mult), r=["ps5"], w=["ncum"])
        self.act(lambda e: e.activation(out=ecum[:, :], in_=ps[5][:, 0:32], func=AF.Exp), r=["ps5"], w=["ecum"])
        self.act(lambda e: e.activation(out=dec[:, :], in_=ps[5][:, 32:64], func=AF.Exp), r=["ps5"], w=["dec"])
        for g in range(4):
            do_group(d, c, cs, g, TRI, NEGM, last, ya, yk)
        if d == 0:
            self.dve(lambda e: e.tensor_tensor(out=yf[:, :].rearrange("p (j q) -> p j q", q=64), in0=xTM[:, c, :].rearrange("p (j q) -> p j q", q=64),
                                               in1=sks[:, :].unsqueeze(2).to_broadcast([128, 32, 64]), op=ALU.mult), r=[("xTM", c), "sks"], w=["yf"])
            self.dve(lambda e: e.tensor_tensor(out=ya[:, :], in0=ya[:, :], in1=yf[:, :], op=ALU.add), r=[yk, "yf"], w=[yk])
            self.dma("sp", self.yfw[col0:col0 + 128, :], ya[:, :], r=[yk])
        else:
            self.dma("sp", yf[:, :], self.yfw[col0:col0 + 128, :], w=["yf"])
            self.dve(lambda e: e.tensor_tensor(out=ya[:, :], in0=ya[:, :], in1=yf[:, :], op=ALU.add), r=[yk, "yf"], w=[yk])
            self.dve(lambda e: e.tensor_tensor(out=ya[:, :], in0=ya[:, :], in1=zTM[:, c, :], op=ALU.mult), r=[yk, ("zTM", c)], w=[yk])
            for g in range(4):
                self.act(lambda e, g=g: e.activation(out=junk[:, :], in_=ya[:, g * 512:(g + 1) * 512], func=AF.Square, accum_out=ssq[:, g:g + 1]),
                         r=[yk], w=["junk", ("ssq", g)])
            self.act(lambda e: e.activation(out=ssq[:, :], in_=ssq[:, :], func=AF.Sqrt, bias=self.epsc[:, 0:1], scale=1.0 / 512.0),
                     r=[("ssq", g_) for g_ in range(4)] + ["epsc"], w=["ssqr"])
            self.dve(lambda e: e.reciprocal(out=ssq[:, :], in_=ssq[:, :]), r=["ssqr"], w=["ssqr"])
            self.dve(lambda e: e.tensor_tensor(out=ya[:, :].rearrange("p (g q) -> p g q", q=512), in0=ya[:, :].rearrange("p (g q) -> p g q", q=512),
                                               in1=ssq[:, :].unsqueeze(2).to_broadcast([128, 4, 512]), op=ALU.mult), r=[yk, "ssqr"], w=[yk])
            self.dve(lambda e: e.tensor_tensor(out=ynb[:, :], in0=ya[:, :], in1=ssmg[:, :], op=ALU.mult), r=[yk, "ssmg"], w=["ynb"])

            def fo(e):
                for t_ in range(8):
                    e.transpose(out=psbA[:, t_ * 128:(t_ + 1) * 128], in_=ynb[:, t_ * 128:(t_ + 1) * 128], identity=identb)
                for t_ in range(8):
                    ins = e.transpose(out=psbB[:, t_ * 128:(t_ + 1) * 128], in_=ynb[:, (8 + t_) * 128:(9 + t_) * 128], identity=identb)
                return ins
            self.pe(fo, r=["ynb", "cb"], w=["ps6", "ps7"])
            of, ok = oFM[yb], ("oFM", yb)
            self.dve(lambda e: e.tensor_copy(out=of[:, 0:8, :], in_=psbA[:, :].rearrange("p (t q) -> p t q", q=128)), r=["ps6"], w=[ok])
            self.act(lambda e: e.activation(out=of[:, 8:16, :], in_=psbB[:, :].rearrange("p (t q) -> p t q", q=128), func=AF.Copy), r=["ps7"], w=[ok])
            self.dma("sp", mixv[:, 16:32, col0:col0 + 128], of[:, :, :], r=[ok])

    def do_group(d, c, cs, g, TRI, NEGM, last, ya, yk):
        b = cnt["g"] % 2
        cnt["g"] += 1
        Ba, r2, La, Ma, cb_, w_, xd, xw, tm = Ball[b], rhs2[b], Lall[b], Mall[b], cbs[b], wv[b], xdt[b], xdw[b], tmp[b]
        gj = slice(g * 8, (g + 1) * 8)
        pa, pb_ = ps[0 + 2 * b], ps[1 + 2 * b]
        pak, pbk = "ps%d" % (2 * b), "ps%d" % (1 + 2 * b)
        self.dve(lambda e: e.tensor_tensor(out=Ba[:, :, :], in0=TRI.unsqueeze(1).to_broadcast([128, 8, 128]),
                                           in1=ld[:, gj].unsqueeze(2).to_broadcast([128, 8, 128]), op=ALU.mult), r=["ld", "cf"], w=[("Ball", b)])
        self.dve(lambda e: e.tensor_tensor(out=r2[:, :, :], in0=NEGM[:, 0:128].unsqueeze(1).to_broadcast([128, 8, 128]),
                                           in1=ncum[:, gj].unsqueeze(2).to_broadcast([128, 8, 128]), op=ALU.add), r=["ncum", "cf"], w=[("rhs2", b)])

        def fl(e):
            for half, pp in ((0, pa), (1, pb_)):
                e.matmul(pp[:, :], onesf, Ba[:, half * 4:(half + 1) * 4, :], start=True, stop=False)
                ins = e.matmul(pp[:, :], identf, r2[:, half * 4:(half + 1) * 4, :], start=False, stop=True)
            return ins
        self.pe(fl, r=[("Ball", b), ("rhs2", b), "cf"], w=[pak, pbk])
        self.act(lambda e: e.activation(out=La[:, 0:4, :], in_=pa[:, :].rearrange("p (j l) -> p j l", l=128), func=AF.Exp), r=[pak], w=[("Lall", b)])
        self.act(lambda e: e.activation(out=La[:, 4:8, :], in_=pb_[:, :].rearrange("p (j l) -> p j l", l=128), func=AF.Exp), r=[pbk], w=[("Lall", b)])
        self.act(lambda e: e.activation(out=w_[:, 0:4], in_=pa[:, :].rearrange("p (j l) -> p j l", l=128)[:, :, last], func=AF.Exp), r=[pak], w=[("wv", b)])
        self.act(lambda e: e.activation(out=w_[:, 4:8], in_=pb_[:, :].rearrange("p (j l) -> p j l", l=128)[:, :, last], func=AF.Exp), r=[pbk], w=[("wv", b)])
        self.pe(lambda e: e.matmul(ps[4][:, 0:128], cvo[:, 16 + g, cs], cvo[:, 20 + g, cs], start=True, stop=True),
                r=[("cvo", 16 + g), ("cvo", 20 + g)], w=["ps4"])
        self.act(lambda e: e.activation(out=cb_[:, :], in_=ps[4][:, 0:128], func=AF.Copy), r=["ps4"], w=[("cbs", b)])
        self.dve(lambda e: e.tensor_tensor(out=Ma[:, :, :], in0=La[:, :, :], in1=cb_[:, :].unsqueeze(1).to_broadcast([128, 8, 128]), op=ALU.mult),
                 r=[("Lall", b), ("cbs", b)], w=[("Mall", b)])
        dts = dtTM[:, c, d * 32 + g * 8:d * 32 + (g + 1) * 8]
        self.dve(lambda e: e.tensor_tensor(out=xd[:, :].rearrange("p (j q) -> p j q", q=64), in0=xTM[:, c, g * 512:(g + 1) * 512].rearrange("p (j q) -> p j q", q=64),
                                           in1=dts.unsqueeze(2).to_broadcast([128, 8, 64]), op=ALU.mult), r=[("xTM", c), ("dtTM", c)], w=[("xdt", b)])
        self.dve(lambda e: e.tensor_tensor(out=xw[:, :].rearrange("p (j q) -> p j q", q=64), in0=xd[:, :].rearrange("p (j q) -> p j q", q=64),
                                           in1=w_[:, :].unsqueeze(2).to_broadcast([128, 8, 64]), op=ALU.mult), r=[("xdt", b), ("wv", b)], w=[("xdw", b)])

        def fy(e):
            for j in range(8):
                e.matmul(pa[:, j * 64:(j + 1) * 64], Ma[:, j, :], xd[:, j * 64:(j + 1) * 64], start=True, stop=True)
            return e.matmul(pb_[:, :], cvo[:, 20 + g, cs], hbf[:, g, :], start=True, stop=True)
        self.pe(fy, r=[("Mall", b), ("xdt", b), ("cvo", 20 + g), ("hbf", g)], w=[pak, pbk])
        self.dve(lambda e: e.tensor_tensor(out=tm[:, :].rearrange("p (j q) -> p j q", q=64), in0=pb_[:, :].rearrange("p (j q) -> p j q", q=64),
                                           in1=ecum[:, gj].unsqueeze(2).to_broadcast([128, 8, 64]), op=ALU.mult), r=[pbk, "ecum"], w=[("ytmp", b)])
        self.dve(lambda e: e.tensor_tensor(out=ya[:, g * 512:(g + 1) * 512], in0=pa[:, :], in1=tm[:, :], op=ALU.add), r=[pak, ("ytmp", b)], w=[yk])
        self.pe(lambda e: e.matmul(ps[4][:, :], BTM[:, c, g * 128:(g + 1) * 128], xw[:, :], start=True, stop=True), r=[("BTM", c), ("xdw", b)], w=["ps4"])
        hg = hst[:, g, :]
        self.dve(lambda e: e.tensor_tensor(out=hg.rearrange("p (j q) -> p j q", q=64), in0=hg.rearrange("p (j q) -> p j q", q=64),
                                           in1=dec[:, gj].unsqueeze(2).to_broadcast([128, 8, 64]), op=ALU.mult), r=[("hst", g), "dec"], w=[("hst", g)])
        self.dve(lambda e: e.tensor_tensor(out=hg, in0=hg, in1=ps[4][:, :], op=ALU.add), r=[("hst", g), "ps4"], w=[("hst", g)])
        self.act(lambda e: e.activation(out=hbf[:, g, :], in_=hg, func=AF.Copy), r=[("hst", g)], w=[("hbf", g)])

    for d in range(2):
        self.dve(lambda e: e.memset(hst[:, :, :], 0.0), w=[("hst", g_) for g_ in range(4)])
        self.dve(lambda e: e.memset(hbf[:, :, :], 0.0), w=[("hbf", g_) for g_ in range(4)])
        seqs = [(TL, TC), (0, TL)]
        for (sg0, slen) in seqs:
            offs = list(range(0, slen, NT))
            if d == 1:
                offs = offs[::-1]
            for off in offs:
                do_tile(d, sg0, slen, off)
    self.release(m0)


MK.ssd = _ssd


T_LAT_FULL = 8192
T_CTX_FULL = 256
_CACHE = {}


def kernel(**inputs):
    from contextlib import ExitStack
    layers = (0, 1, 2, 3)
    B = int(np.asarray(inputs["x"]).shape[0])
    consts = make_consts(T_LAT_FULL)
    with ExitStack() as stack:
        mk = MK(T_LAT_FULL, T_CTX_FULL, layers=layers, final=True)
        nc = mk.build(stack)
        in_maps = [prep_inputs(inputs, b, layers, T_LAT_FULL, T_CTX_FULL, consts) for b in range(B)]
        res = run_bass_kernel_spmd(nc, in_maps, core_ids=list(range(B)))
    out = np.stack([np.ascontiguousarray(res.results[b]["outT"].T) for b in range(B)], axis=0)
    return out.astype(np.float32)
```
